# Optimizing a Trainium2 kernel written in Bass

```python
import jax, jax.numpy as jnp
from jax import lax
import numpy as np

D_MODEL = 2048
BATCH = 4
SEQ = 2048
DEPTH = 4

N_MIXERS = 2
EPS = 1e-6
D_FF = ((8 * D_MODEL // 3 + 127) // 128) * 128
ML_HEADS = 4
ML_QK_DIM = D_MODEL // (2 * ML_HEADS)
ML_V_DIM = D_MODEL // ML_HEADS
ML_QK_TOT = ML_HEADS * ML_QK_DIM
ML_V_TOT = ML_HEADS * ML_V_DIM
ML_IN = 2 * ML_QK_TOT + 2 * ML_V_TOT + 2 * ML_HEADS
ML_CHUNK = 64
GATE_SOFTCAP = 15.0
GDN_K_DIM = 128
GDN_V_DIM = 128
GDN_K_HEADS = D_MODEL // 128
GDN_V_HEADS = 2 * GDN_K_HEADS
GDN_K_TOT = GDN_K_HEADS * GDN_K_DIM
GDN_V_TOT = GDN_V_HEADS * GDN_V_DIM
GDN_CONV_CH = 2 * GDN_K_TOT + GDN_V_TOT
GDN_IN = GDN_CONV_CH + GDN_V_TOT + 2 * GDN_V_HEADS
GDN_CONV = 4
GDN_CHUNK = 64
N_ML_LAYERS = (DEPTH + 1) // 2
N_GDN_LAYERS = DEPTH // 2

kernel_name = "interleaved_mlstm_gdn_macaron_trunk"


def rms_norm(x, g):
    xf = x.astype(jnp.float32)
    y = xf * lax.rsqrt(jnp.mean(xf * xf, axis=-1, keepdims=True) + EPS)
    return (y * g.astype(jnp.float32)).astype(x.dtype)


def l2_norm(x):
    return x * lax.rsqrt(jnp.sum(x * x, axis=-1, keepdims=True) + EPS)


def softcap(x):
    return GATE_SOFTCAP * jnp.tanh(x / GATE_SOFTCAP)


def swiglu_ffn(x, w_in, w_out):
    gate, up = jnp.split(x @ w_in, 2, axis=-1)
    return (jax.nn.silu(gate) * up) @ w_out


def mlstm_chunkwise(q, k, v, i_pre, f_log):
    B, H, S, dqk = q.shape
    dv = v.shape[-1]
    L = ML_CHUNK
    NC = S // L

    def to_chunks(a):
        return jnp.moveaxis(a.reshape(B, H, NC, L, *a.shape[3:]), 2, 0)

    xs = tuple(to_chunks(a) for a in (q * (dqk ** -0.5), k, v, i_pre, f_log))
    causal = jnp.tril(jnp.ones((L, L), dtype=bool))

    def step(carry, inp):
        C, n, m = carry
        qb, kb, vb, ib, fb = inp
        b = jnp.cumsum(fb, axis=-1)
        log_d = jnp.where(causal, b[..., :, None] - b[..., None, :] + ib[..., None, :], -jnp.inf)
        log_inter = b + m[..., None]
        m_t = jnp.maximum(log_inter, jnp.max(log_d, axis=-1))
        d = jnp.exp(log_d - m_t[..., None])
        inter = jnp.exp(log_inter - m_t)
        s = jnp.einsum('bhld,bhsd->bhls', qb, kb) * d
        num = inter[..., None] * jnp.einsum('bhld,bhde->bhle', qb, C) + jnp.einsum('bhls,bhse->bhle', s, vb)
        den = inter * jnp.einsum('bhld,bhd->bhl', qb, n) + jnp.sum(s, axis=-1)
        h = num / jnp.maximum(jnp.abs(den), jnp.exp(-m_t))[..., None]
        b_last = b[..., -1]
        log_w = b_last[..., None] - b + ib
        m_new = jnp.maximum(b_last + m, jnp.max(log_w, axis=-1))
        w = jnp.exp(log_w - m_new[..., None])
        decay = jnp.exp(b_last + m - m_new)
        kw = kb * w[..., None]
        C_new = decay[..., None, None] * C + jnp.einsum('bhsd,bhse->bhde', kw, vb)
        n_new = decay[..., None] * n + jnp.sum(kw, axis=2)
        return (C_new, n_new, m_new), h

    init = (jnp.zeros((B, H, dqk, dv), q.dtype), jnp.zeros((B, H, dqk), q.dtype), jnp.zeros((B, H), q.dtype))
    _, hc = lax.scan(step, init, xs)
    return jnp.moveaxis(hc, 0, 2).reshape(B, H, S, dv)


def mlstm_mixer(x, w_in, i_bias, f_bias, head_g, w_out):
    B, S, _ = x.shape
    f32 = jnp.float32
    proj = x @ w_in
    q, k, v, o, i_pre, f_pre = jnp.split(
        proj, [ML_QK_TOT, 2 * ML_QK_TOT, 2 * ML_QK_TOT + ML_V_TOT, 2 * ML_QK_TOT + 2 * ML_V_TOT,
               2 * ML_QK_TOT + 2 * ML_V_TOT + ML_HEADS], axis=-1)

    def heads(a, d):
        return a.reshape(B, S, ML_HEADS, d).transpose(0, 2, 1, 3).astype(f32)

    i_log = softcap(i_pre.astype(f32) + i_bias.astype(f32)).transpose(0, 2, 1)
    f_log = jax.nn.log_sigmoid(softcap(f_pre.astype(f32) + f_bias.astype(f32))).transpose(0, 2, 1)
    h = mlstm_chunkwise(heads(q, ML_QK_DIM), heads(k, ML_QK_DIM), heads(v, ML_V_DIM), i_log, f_log)
    h = rms_norm(h.transpose(0, 2, 1, 3), head_g)
    y = jax.nn.sigmoid(o) * h.reshape(B, S, ML_V_TOT).astype(x.dtype)
    return y @ w_out


def causal_short_conv(x, w):
    S = x.shape[1]
    K = w.shape[0]
    xp = jnp.pad(x, ((0, 0), (K - 1, 0), (0, 0)))
    y = xp[:, 0:S] * w[0]
    for j in range(1, K):
        y = y + xp[:, j:j + S] * w[j]
    return y


def gated_delta_chunked(q, k, v, g, beta):
    B, S, H, dk = q.shape
    dv = v.shape[-1]
    L = GDN_CHUNK
    NC = S // L

    def chunks(a):
        a = jnp.moveaxis(a, 2, 1)
        return a.reshape(B, H, NC, L, *a.shape[3:])

    q, k, v, g, beta = (chunks(a) for a in (q, k, v, g, beta))
    g = jnp.cumsum(g, axis=-1)
    incl = jnp.tril(jnp.ones((L, L), dtype=bool))
    strict = jnp.tril(jnp.ones((L, L), dtype=bool), k=-1)
    decay = jnp.exp(jnp.where(incl, g[..., :, None] - g[..., None, :], -jnp.inf))
    kb = k * beta[..., None]
    vb = v * beta[..., None]
    a_low = jnp.where(strict, jnp.einsum('bhnld,bhnsd->bhnls', kb, k) * decay, 0.0)
    eye = jnp.eye(L, dtype=q.dtype)
    rhs = jnp.concatenate([vb, kb * jnp.exp(g)[..., None]], axis=-1)
    sol = lax.linalg.triangular_solve(eye + a_low, rhs, left_side=True, lower=True, unit_diagonal=True)
    u, w = sol[..., :dv], sol[..., dv:]
    qk = jnp.where(incl, jnp.einsum('bhnld,bhnsd->bhnls', q, k) * decay, 0.0)

    def step(state, inp):
        qb, kb_, ub, wb, gb, ab = inp
        v_new = ub - jnp.einsum('bhld,bhde->bhle', wb, state)
        out = jnp.einsum('bhld,bhde->bhle', qb * jnp.exp(gb)[..., None], state) + jnp.einsum('bhls,bhse->bhle', ab, v_new)
        g_last = gb[..., -1]
        k_dec = kb_ * jnp.exp(g_last[..., None] - gb)[..., None]
        state = state * jnp.exp(g_last)[..., None, None] + jnp.einsum('bhld,bhle->bhde', k_dec, v_new)
        return state, out

    xs = tuple(jnp.moveaxis(a, 2, 0) for a in (q, k, u, w, g, qk))
    _, out = lax.scan(step, jnp.zeros((B, H, dk, dv), q.dtype), xs)
    out = jnp.moveaxis(out, 0, 2).reshape(B, H, S, dv)
    return jnp.moveaxis(out, 1, 2)


def gdn_mixer(x, w_in, conv_w, a_log, dt_bias, norm_g, w_out):
    B, S, _ = x.shape
    f32 = jnp.float32
    proj = x @ w_in
    qkv, z, b_pre, a_pre = jnp.split(proj, [GDN_CONV_CH, GDN_CONV_CH + GDN_V_TOT, GDN_CONV_CH + GDN_V_TOT + GDN_V_HEADS], axis=-1)
    qkv = jax.nn.silu(causal_short_conv(qkv, conv_w))
    q, k, v = jnp.split(qkv, [GDN_K_TOT, 2 * GDN_K_TOT], axis=-1)
    q = l2_norm(q.reshape(B, S, GDN_K_HEADS, GDN_K_DIM).astype(f32)) * (GDN_K_DIM ** -0.5)
    k = l2_norm(k.reshape(B, S, GDN_K_HEADS, GDN_K_DIM).astype(f32))
    rep = GDN_V_HEADS // GDN_K_HEADS
    q = jnp.repeat(q, rep, axis=2)
    k = jnp.repeat(k, rep, axis=2)
    v = v.reshape(B, S, GDN_V_HEADS, GDN_V_DIM).astype(f32)
    beta = jax.nn.sigmoid(b_pre.astype(f32))
    g = -jnp.exp(a_log.astype(f32)) * jax.nn.softplus(a_pre.astype(f32) + dt_bias.astype(f32))
    o = gated_delta_chunked(q, k, v, g, beta)
    o = rms_norm(o, norm_g).astype(x.dtype) * jax.nn.silu(z.reshape(B, S, GDN_V_HEADS, GDN_V_DIM))
    return o.reshape(B, S, GDN_V_TOT) @ w_out


def setup_inputs(seed: int = 0) -> dict:
    key = jax.random.key(seed)
    ks = jax.random.split(key, 16)
    nrm = jax.random.normal
    x = nrm(ks[0], (BATCH, SEQ, D_MODEL), jnp.float32)
    norm_g = 1.0 + 0.02 * nrm(ks[1], (DEPTH, 6, D_MODEL), jnp.float32)
    ffn_w_in = nrm(ks[2], (DEPTH, 2, D_MODEL, 2 * D_FF), jnp.float32) * D_MODEL ** -0.5
    ffn_w_out = nrm(ks[3], (DEPTH, 2, D_FF, D_MODEL), jnp.float32) * D_FF ** -0.5
    ml_w_in = nrm(ks[4], (N_ML_LAYERS, D_MODEL, ML_IN), jnp.float32) * D_MODEL ** -0.5
    ml_i_bias = 0.1 * nrm(ks[5], (N_ML_LAYERS, ML_HEADS), jnp.float32)
    ml_f_bias = 3.0 + 3.0 * jax.random.uniform(ks[6], (N_ML_LAYERS, ML_HEADS), jnp.float32)
    ml_head_g = 1.0 + 0.02 * nrm(ks[7], (N_ML_LAYERS, ML_HEADS, ML_V_DIM), jnp.float32)
    ml_w_out = nrm(ks[8], (N_ML_LAYERS, ML_V_TOT, D_MODEL), jnp.float32) * ML_V_TOT ** -0.5
    gdn_w_in = nrm(ks[9], (N_GDN_LAYERS, D_MODEL, GDN_IN), jnp.float32) * D_MODEL ** -0.5
    gdn_conv_w = nrm(ks[10], (N_GDN_LAYERS, GDN_CONV, GDN_CONV_CH), jnp.float32) * GDN_CONV ** -0.5
    gdn_a_log = jnp.log(jax.random.uniform(ks[11], (N_GDN_LAYERS, GDN_V_HEADS), jnp.float32, 1.0, 16.0))
    dt = jnp.exp(jax.random.uniform(ks[12], (N_GDN_LAYERS, GDN_V_HEADS), jnp.float32, jnp.log(0.001), jnp.log(0.1)))
    gdn_dt_bias = dt + jnp.log(-jnp.expm1(-dt))
    gdn_norm_g = 1.0 + 0.02 * nrm(ks[13], (N_GDN_LAYERS, GDN_V_DIM), jnp.float32)
    gdn_w_out = nrm(ks[14], (N_GDN_LAYERS, GDN_V_TOT, D_MODEL), jnp.float32) * GDN_V_TOT ** -0.5
    return {"x": x, "norm_g": norm_g, "ffn_w_in": ffn_w_in, "ffn_w_out": ffn_w_out,
            "ml_w_in": ml_w_in, "ml_i_bias": ml_i_bias, "ml_f_bias": ml_f_bias, "ml_head_g": ml_head_g,
            "ml_w_out": ml_w_out, "gdn_w_in": gdn_w_in, "gdn_conv_w": gdn_conv_w, "gdn_a_log": gdn_a_log,
            "gdn_dt_bias": gdn_dt_bias, "gdn_norm_g": gdn_norm_g, "gdn_w_out": gdn_w_out}


def reference(x, norm_g, ffn_w_in, ffn_w_out, ml_w_in, ml_i_bias, ml_f_bias, ml_head_g, ml_w_out,
              gdn_w_in, gdn_conv_w, gdn_a_log, gdn_dt_bias, gdn_norm_g, gdn_w_out):
    h = x
    for layer in range(DEPTH):
        g = norm_g[layer]
        h = h + 0.5 * rms_norm(swiglu_ffn(rms_norm(h, g[0]), ffn_w_in[layer, 0], ffn_w_out[layer, 0]), g[1])
        j = layer // N_MIXERS
        hn = rms_norm(h, g[2])
        if layer % N_MIXERS == 0:
            mix = mlstm_mixer(hn, ml_w_in[j], ml_i_bias[j], ml_f_bias[j], ml_head_g[j], ml_w_out[j])
        else:
            mix = gdn_mixer(hn, gdn_w_in[j], gdn_conv_w[j], gdn_a_log[j], gdn_dt_bias[j], gdn_norm_g[j], gdn_w_out[j])
        h = h + rms_norm(mix, g[3])
        h = h + 0.5 * rms_norm(swiglu_ffn(rms_norm(h, g[4]), ffn_w_in[layer, 1], ffn_w_out[layer, 1]), g[5])
    return h
```

```python
import numpy as np
from contextlib import ExitStack

import concourse.bass as bass
import concourse.mybir as mybir
from concourse.bass_utils import run_bass_kernel_spmd

F32 = mybir.dt.float32
BF16 = mybir.dt.bfloat16
AF = mybir.ActivationFunctionType
ALU = mybir.AluOpType
AX = mybir.AxisListType

D = 2048
DC = D // 128
DFF = 5504
FC = DFF // 128
EPS = 1e-6
NCORES = 8
SEQ = 2048
TOK = 1024
TT = 512

ENGS = ("pe", "act", "dve", "pool", "sp")
SEM_CHUNK = 30000
NSLOT = 8


class Dummy:
    def __getitem__(self, k):
        return self

    def __getattr__(self, k):
        return self

    def __call__(self, *a, **k):
        return self


DUMMY = Dummy()


class Buf:
    __slots__ = ("name", "lw", "rd", "excl")

    def __init__(self, name, excl=False):
        self.name = name
        self.excl = excl
        self.lw = None
        self.rd = {}


class StopBuild(Exception):
    pass


class Prog:
    limit = None

    def __init__(self, nc=None, needs=None):
        self.nc = nc
        self.dry = nc is None
        self.needs = needs if needs is not None else set()
        self.nop = 0
        self.seen = {e: {} for e in ENGS}
        self.last_op = {e: None for e in ENGS}
        self.ticket = {e: 0 for e in ENGS}
        self.op_ticket = {}
        self.dslot_next = {"sp": 0, "pool": 0}
        self.dslot_cnt = {(q, s): 0 for q in ("sp", "pool") for s in range(NSLOT)}
        self.es = ExitStack()
        self.scopes = []
        self.nalloc = 0
        if not self.dry:
            self.E = {"pe": nc.tensor, "act": nc.scalar, "dve": nc.vector, "pool": nc.gpsimd, "sp": nc.sync}
            self.esems = {e: [] for e in ENGS}
            self.dsems = {}
            for q in ("sp", "pool"):
                for s in range(NSLOT):
                    self.dsems[(q, s)] = self.es.enter_context(nc.semaphore(f"d_{q}_{s}"))

    def push(self):
        self.scopes.append(ExitStack())

    def pop(self):
        self.barrier()
        self.scopes.pop().close()

    def sb(self, name, shape, dt):
        self.nalloc += 1
        if self.dry:
            return DUMMY
        return self.scopes[-1].enter_context(self.nc.sbuf_tensor(f"{name}_{self.nalloc}", list(shape), dt))

    def ps(self, name, shape, dt=F32):
        self.nalloc += 1
        if self.dry:
            return DUMMY
        return self.scopes[-1].enter_context(self.nc.psum_tensor(f"{name}_{self.nalloc}", list(shape), dt))

    def dram(self, name, shape, dt, kind="Internal"):
        if self.dry:
            return DUMMY
        return self.nc.dram_tensor(name, list(shape), dt, kind=kind).ap()

    def _esem(self, eng, t):
        idx = (t - 1) // SEM_CHUNK
        lst = self.esems[eng]
        while len(lst) <= idx:
            lst.append(self.es.enter_context(self.nc.semaphore(f"e_{eng}_{len(lst)}")))
        return lst[idx], (t - 1) % SEM_CHUNK + 1

    def _wait(self, eng, tok):
        if tok is None:
            return
        if tok[0] == "e":
            _, src, opid = tok
            if src == eng and eng == "pe":
                return
            if self.seen[eng].get(src, -1) >= opid:
                return
            self.seen[eng][src] = opid
            if self.dry:
                self.needs.add(opid)
            else:
                t = self.op_ticket[opid]
                sem, val = self._esem(src, t)
                self.E[eng].wait_ge(sem, val)
        else:
            _, q, slot, cnt = tok
            key = (q, slot)
            if self.seen[eng].get(key, 0) >= cnt:
                return
            self.seen[eng][key] = cnt
            if not self.dry:
                self.E[eng].wait_ge(self.dsems[key], cnt)

    def _deps(self, eng, r, w):
        for b in r:
            self._wait(eng, b.lw)
            if b.excl:
                for k, tok in b.rd.items():
                    if k != eng:
                        self._wait(eng, tok)
        for b in w:
            self._wait(eng, b.lw)
            for tok in b.rd.values():
                self._wait(eng, tok)

    def _mark(self, tok, r, w, rkey):
        for b in r:
            b.rd[rkey] = tok
        for b in w:
            b.lw = tok
            b.rd = {}

    def op(self, eng, fn, r=(), w=()):
        if Prog.limit is not None and self.nop >= Prog.limit:
            raise StopBuild()
        opid = self.nop
        self.nop += 1
        self._deps(eng, r, w)
        tok = ("e", eng, opid)
        if not self.dry:
            inst = fn(self.E[eng])
            if opid in self.needs:
                self.ticket[eng] += 1
                t = self.ticket[eng]
                sem, _ = self._esem(eng, t)
                inst.then_inc(sem, 1)
                self.op_ticket[opid] = t
        self.last_op[eng] = tok
        self._mark(tok, r, w, eng)
        return tok

    def dma(self, q, out, in_, r=(), w=()):
        self.nop += 1
        self._deps(q, r, w)
        slot = self.dslot_next[q]
        self.dslot_next[q] = (slot + 1) % NSLOT
        key = (q, slot)
        prev = self.dslot_cnt[key]
        if prev:
            self._wait(q, ("d", q, slot, prev))
        cnt = prev + 16
        self.dslot_cnt[key] = cnt
        tok = ("d", q, slot, cnt)
        if not self.dry:
            self.E[q].dma_start(out=out(), in_=in_()).then_inc(self.dsems[key], 16)
        self._mark(tok, r, w, ("d", q, slot))
        return tok

    def barrier(self):
        toks = [self.last_op[e] for e in ENGS if self.last_op[e] is not None]
        dtoks = [("d", q, s, c) for (q, s), c in self.dslot_cnt.items() if c]
        for e in ENGS:
            for t in toks:
                if t[1] != e:
                    self._wait(e, t)
            for t in dtoks:
                self._wait(e, t)

    def finish(self):
        self.barrier()


class Ctx:
    def __init__(self):
        self.btiles = {}

    def bias_tile(self, P, val):
        key = float(val)
        if key not in self.btiles:
            t = P.sb("bias", [128, 1], F32)
            P.op("pool", lambda e: e.memset(t[:], key), w=[self.b_const])
            self.btiles[key] = t
        return self.btiles[key]


def setup_common(P, cx, consts_ap, npsum=8):
    cx.ones_bf = P.sb("ones_bf", [128, 128], BF16)
    cx.ident_bf = P.sb("ident_bf", [128, 128], BF16)
    cx.ident_f = P.sb("ident_f", [128, 128], F32)
    cx.b_const = Buf("consts")
    P.dma("pool", lambda: cx.ones_bf[:], lambda: consts_ap[:, 0:128], w=[cx.b_const])
    P.dma("pool", lambda: cx.ident_bf[:], lambda: consts_ap[:, 128:256], w=[cx.b_const])
    P.dma("sp", lambda: cx.ident_f[:], lambda: consts_ap[:, 128:256], w=[cx.b_const])
    cx.psum = [P.ps(f"ps{i}", [128, 512]) for i in range(npsum)]
    cx.bps = [Buf(f"ps{i}", excl=True) for i in range(npsum)]


def rms_stats(P, cx, src_fn, nchunk, ntok, ps_i, sq, b_sq, b_src, rstd, b_rstd, scale_mean, post_mul=None):
    ps = cx.psum[ps_i]
    bp = cx.bps[ps_i]
    for c in range(nchunk):
        k = c % 2
        P.op("act", lambda e, c=c, k=k: e.activation(out=sq[k][:, :ntok], in_=src_fn(c), func=AF.Square),
             r=[b_src], w=[b_sq[k]])
        P.op("pe", lambda e, c=c, k=k: e.matmul(ps[:, :ntok], cx.ones_bf[:], sq[k][:, :ntok],
                                                 start=(c == 0), stop=(c == nchunk - 1)),
             r=[b_sq[k], cx.b_const], w=[bp])
    pm = 1.0 if post_mul is None else float(post_mul)
    bt = cx.bias_tile(P, EPS / (pm * pm))
    P.op("act", lambda e: e.activation(out=rstd[:, :ntok], in_=ps[:, :ntok], func=AF.Sqrt, bias=bt[:, 0:1],
                                       scale=float(scale_mean / (pm * pm))),
         r=[bp, cx.b_const], w=[b_rstd])
    P.op("dve", lambda e: e.reciprocal(out=rstd[:, :ntok], in_=rstd[:, :ntok]), r=[b_rstd], w=[b_rstd])


def emit_ffn(P, cx, h, b_h, gvec, b_g, gi_pre, gi_post, w_in_ap, w_out_ap, S):
    for tt in range(TOK // TT):
        tsl = slice(tt * TT, (tt + 1) * TT)
        rms_stats(P, cx, lambda c: h[:, c, tsl], DC, TT, 6, S.sq, S.b_sq, b_h, S.rstd, S.b_rstd, 1.0 / D)
        for c in range(DC):
            P.op("dve", lambda e, c=c: e.scalar_tensor_tensor(out=S.xn[:, c, :], in0=h[:, c, tsl],
                                                             scalar=gvec[:, gi_pre, c:c + 1], in1=S.rstd[:, :],
                                                             op0=ALU.mult, op1=ALU.mult),
                 r=[b_h, b_g, S.b_rstd], w=[S.b_xn])
        for j in range(FC):
            k = S.wk % 3
            S.wk += 1
            wt = S.wbuf[k]
            P.dma("pool", lambda wt=wt: wt[:, 0:DC * 256], lambda j=j: w_in_ap[j].rearrange("p c g m -> p (c g m)"),
                  w=[S.b_w[k]])
            pg = cx.psum[j % 2]
            pu = cx.psum[2 + j % 2]
            for c in range(DC):
                P.op("pe", lambda e, c=c, wt=wt, pg=pg: e.matmul(pg[:, :], wt[:, c * 256:c * 256 + 128], S.xn[:, c, :],
                                                                start=(c == 0), stop=(c == DC - 1)),
                     r=[S.b_w[k], S.b_xn], w=[cx.bps[j % 2]])
            for c in range(DC):
                P.op("pe", lambda e, c=c, wt=wt, pu=pu: e.matmul(pu[:, :], wt[:, c * 256 + 128:c * 256 + 256], S.xn[:, c, :],
                                                                start=(c == 0), stop=(c == DC - 1)),
                     r=[S.b_w[k], S.b_xn], w=[cx.bps[2 + j % 2]])
            sg = S.sg[j % 2]
            P.op("act", lambda e, sg=sg, pg=pg: e.activation(out=sg[:, :], in_=pg[:, :], func=AF.Silu),
                 r=[cx.bps[j % 2]], w=[S.b_sg[j % 2]])
            P.op("dve", lambda e, j=j, sg=sg, pu=pu: e.tensor_tensor(out=S.act[:, j, :], in0=sg[:, :], in1=pu[:, :],
                                                                    op=ALU.mult),
                 r=[S.b_sg[j % 2], cx.bps[2 + j % 2]], w=[S.b_act])
        emit_outproj_resid(P, cx, h, b_h, tsl, gvec, b_g, gi_post, w_out_ap, FC, S, 0.5)


def emit_outproj_resid(P, cx, h, b_h, tsl, gvec, b_g, gi_post, w_out_ap, nk, S, post_mul):
    for m in range(DC):
        k = S.wk % 3
        S.wk += 1
        wt = S.wbuf[k]
        P.dma("pool", lambda wt=wt: wt[:, 0:nk * 128], lambda m=m: w_out_ap[m].rearrange("p c m -> p (c m)"),
              w=[S.b_w[k]])
        py = cx.psum[4 + m % 2]
        for c in range(nk):
            P.op("pe", lambda e, c=c, wt=wt, py=py: e.matmul(py[:, :], wt[:, c * 128:(c + 1) * 128], S.act[:, c, :],
                                                            start=(c == 0), stop=(c == nk - 1)),
                 r=[S.b_w[k], S.b_act], w=[cx.bps[4 + m % 2]])
        P.op("act", lambda e, m=m, py=py: e.activation(out=S.y[:, m, :], in_=py[:, :], func=AF.Copy),
             r=[cx.bps[4 + m % 2]], w=[S.b_y])
    rms_stats(P, cx, lambda c: S.y[:, c, :], DC, TT, 6, S.sq, S.b_sq, S.b_y, S.rstd, S.b_rstd, 1.0 / D,
              post_mul=post_mul)
    for c in range(DC):
        P.op("dve", lambda e, c=c: e.scalar_tensor_tensor(out=S.y[:, c, :], in0=S.y[:, c, :],
                                                         scalar=gvec[:, gi_post, c:c + 1], in1=S.rstd[:, :],
                                                         op0=ALU.mult, op1=ALU.mult),
             r=[b_g, S.b_rstd], w=[S.b_y])
        P.op("pool", lambda e, c=c: e.tensor_tensor(out=h[:, c, tsl], in0=h[:, c, tsl], in1=S.y[:, c, :],
                                                   op=ALU.add),
             r=[S.b_y], w=[b_h])


def emit_mixout(P, cx, h, b_h, gvec, b_g, gi_post, yT_ap, nk, w_out_ap, S):
    for tt in range(TOK // TT):
        tsl = slice(tt * TT, (tt + 1) * TT)
        for half in range(2):
            hs = slice(half * (nk // 2), (half + 1) * (nk // 2))
            P.dma("pool", lambda hs=hs: S.act[:, hs, :],
                  lambda hs=hs, tsl=tsl: yT_ap[hs.start * 128:hs.stop * 128, tsl].rearrange("(c p) t -> p c t", p=128),
                  w=[S.b_act])
        emit_outproj_resid(P, cx, h, b_h, tsl, gvec, b_g, gi_post, w_out_ap, nk, S, None)


class FfnScratch:
    def __init__(self, P):
        self.sq = [P.sb(f"sq{i}", [128, TT], BF16) for i in range(2)]
        self.b_sq = [Buf(f"sq{i}") for i in range(2)]
        self.rstd = P.sb("rstd", [128, TT], F32)
        self.b_rstd = Buf("rstd")
        self.xn = P.sb("xn", [128, DC, TT], BF16)
        self.b_xn = Buf("xn")
        self.act = P.sb("act", [128, FC, TT], BF16)
        self.b_act = Buf("act")
        self.y = P.sb("y", [128, DC, TT], F32)
        self.b_y = Buf("y")
        self.sg = [P.sb(f"sg{i}", [128, TT], F32) for i in range(2)]
        self.b_sg = [Buf(f"sg{i}") for i in range(2)]
        self.wbuf = [P.sb(f"wbuf{i}", [128, FC * 128], BF16) for i in range(3)]
        self.b_w = [Buf(f"w{i}") for i in range(3)]
        self.wk = 0


def build_tok_program(P, stages):
    cx = Ctx()
    hT_in = P.dram("hT_in", [D, TOK], F32, kind="ExternalInput")
    hT_out = P.dram("hT_out", [D, TOK], F32, kind="ExternalOutput")
    consts = P.dram("consts", [128, 256], F32, kind="ExternalInput")
    gains = P.dram("gains", [128, 24, DC], F32, kind="ExternalInput")
    aps = []
    for i, st in enumerate(stages):
        if st[0] == "ffn":
            aps.append((P.dram(f"w_in_{i}", [FC, 128, DC, 2, 128], F32, kind="ExternalInput"),
                        P.dram(f"w_out_{i}", [DC, 128, FC, 128], F32, kind="ExternalInput")))
        else:
            nk = st[2]
            aps.append((P.dram(f"yT_{i}", [nk * 128, TOK], F32, kind="ExternalInput"),
                        P.dram(f"w_out_{i}", [DC, 128, nk, 128], F32, kind="ExternalInput")))
    setup_common(P, cx, consts)
    h = P.sb("h", [128, DC, TOK], F32)
    b_h = Buf("h")
    gvec = P.sb("gvec", [128, 24, DC], F32)
    b_g = Buf("g")
    P.dma("sp", lambda: gvec[:], lambda: gains[:], w=[b_g])
    for half in range(2):
        P.dma("sp", lambda half=half: h[:, half * 8:(half + 1) * 8, :],
              lambda half=half: hT_in[half * 1024:(half + 1) * 1024, :].rearrange("(c p) t -> p c t", p=128), w=[b_h])
    S = FfnScratch(P)
    for i, st in enumerate(stages):
        if st[0] == "ffn":
            emit_ffn(P, cx, h, b_h, gvec, b_g, st[1], st[2], aps[i][0], aps[i][1], S)
        else:
            emit_mixout(P, cx, h, b_h, gvec, b_g, st[1], aps[i][0], st[2], aps[i][1], S)
    b_out = Buf("out")
    for half in range(2):
        P.dma("sp", lambda half=half: hT_out[half * 1024:(half + 1) * 1024, :].rearrange("(c p) t -> p c t", p=128),
              lambda half=half: h[:, half * 8:(half + 1) * 8, :], r=[b_h], w=[b_out])
    P.finish()
    P.pop()
    P.es.close()


def make_program(builder, *args):
    dry = Prog(None)
    dry.push()
    builder(dry, *args)
    nc = bass.Bass("TRN2", target_bir_lowering=False)
    real = Prog(nc, needs=dry.needs)
    real.push()
    builder(real, *args)
    return nc


def consts_np():
    c = np.zeros((128, 256), np.float32)
    c[:, 0:128] = 1.0
    c[:, 128:256] = np.eye(128, dtype=np.float32)
    return c


def lay_w_in(w):
    g = w[:, :DFF].reshape(DC, 128, FC, 128)
    u = w[:, DFF:].reshape(DC, 128, FC, 128)
    a = np.stack([g, u], axis=0)
    return np.ascontiguousarray(a.transpose(3, 2, 1, 0, 4))


def lay_w_out(w):
    a = w.reshape(w.shape[0] // 128, 128, DC, 128)
    return np.ascontiguousarray(a.transpose(2, 1, 0, 3))


def lay_gain(g):
    n = g.shape[0]
    return np.ascontiguousarray(g.reshape(n, DC, 128).transpose(2, 0, 1))


ML_DQK = 256
ML_DV = 512
SEG = 512
NEG = -30000.0


def proj_fm(P, cx, wt, col0, xn, b_w, b_xn, ps_i, nk=DC, ntok=SEG, ncol=128, stride=None):
    ps = cx.psum[ps_i]
    for c in range(nk):
        P.op("pe", lambda e, c=c: e.matmul(ps[:ncol, :ntok], wt[:, c, col0:col0 + ncol], xn[:, c, :ntok],
                                           start=(c == 0), stop=(c == nk - 1)),
             r=[b_w, b_xn], w=[cx.bps[ps_i]])
    return ps


def build_mlstm_program(P, nseg=SEQ // SEG):
    cx = Ctx()
    hT_full = P.dram("hT_full", [D, SEQ], F32, kind="ExternalInput")
    consts = P.dram("consts", [128, 256], F32, kind="ExternalInput")
    consts2 = P.dram("consts2", [128, 128 + 256], F32, kind="ExternalInput")
    gains = P.dram("gains", [128, 1, DC], F32, kind="ExternalInput")
    wqk = P.dram("wqk", [2, 128, DC, 512], F32, kind="ExternalInput")
    wv = P.dram("wv", [2, 128, DC, 512], F32, kind="ExternalInput")
    wo = P.dram("wo", [2, 128, DC, 512], F32, kind="ExternalInput")
    wif = P.dram("wif", [128, DC, 4], F32, kind="ExternalInput")
    gbias = P.dram("gbias", [2, 2], F32, kind="ExternalInput")
    headg = P.dram("headg", [128, 8], F32, kind="ExternalInput")
    yT = P.dram("yT", [1024, SEQ], F32, kind="ExternalOutput")
    setup_common(P, cx, consts)
    sb = P.sb
    maskb = sb("maskb", [128, 128], F32)
    sel = sb("sel", [2, 256], F32)
    ones_f = sb("ones_f", [2, SEG], F32)
    gv = sb("gv", [128, 1, DC], F32)
    hg = sb("hg", [128, 8], F32)
    wif_sb = sb("wif", [128, DC, 4], BF16)
    gb = sb("gb", [2, 2], F32)
    gb15 = sb("gb15", [2, 2], F32)
    b_c2 = Buf("c2")
    P.dma("sp", lambda: maskb[:], lambda: consts2[:, 0:128], w=[b_c2])
    P.dma("sp", lambda: sel[:], lambda: consts2[0:2, 128:384], w=[b_c2])
    P.dma("sp", lambda: gv[:], lambda: gains[:], w=[b_c2])
    P.dma("sp", lambda: hg[:], lambda: headg[:], w=[b_c2])
    P.dma("pool", lambda: wif_sb[:], lambda: wif[:], w=[b_c2])
    P.dma("sp", lambda: gb[:], lambda: gbias[:], w=[b_c2])
    P.op("pool", lambda e: e.memset(ones_f[:], 1.0), w=[b_c2])
    P.op("act", lambda e: e.mul(out=gb15[:], in_=gb[:], mul=1.0 / 15.0), r=[b_c2], w=[b_c2])
    one_t = cx.bias_tile(P, 1.0)

    hseg = sb("hseg", [128, DC, SEG], F32); b_hseg = Buf("hseg")
    xn = sb("xn", [128, DC, SEG], BF16); b_xn = Buf("xn")
    sq = [sb(f"sq{i}", [128, SEG], BF16) for i in range(2)]; b_sq = [Buf("sq0"), Buf("sq1")]
    rstd = sb("rstd", [128, SEG], F32); b_rstd = Buf("rstd")
    wb = [sb(f"wb{i}", [128, DC, 512], BF16) for i in range(2)]; b_wb = [Buf(f"wb{i}") for i in range(2)]
    qT = sb("qT", [128, 4, SEG], BF16); b_qT = Buf("qT")
    kT = sb("kT", [128, 4, SEG], BF16); b_kT = Buf("kT")
    ktok = sb("ktok", [128, 4, 512], F32); b_ktok = Buf("ktok")
    vtok = sb("vtok", [128, 4, 2, 512], BF16); b_vtok = Buf("vtok")
    og = sb("og", [128, 8, SEG], BF16); b_og = Buf("og")
    hn = sb("hn", [128, 8, SEG], F32); b_hn = Buf("hn")
    gi = sb("gi", [2, SEG], F32); gf = sb("gf", [2, SEG], F32)
    Bt = sb("Bt", [2, SEG], F32); al = sb("al", [2, SEG], F32); Gt = sb("Gt", [2, SEG], F32)
    Gp = sb("Gp", [2, SEG], F32); Gl = sb("Gl", [2, SEG], F32)
    r_int = sb("r_int", [2, SEG], F32); r_em = sb("r_em", [2, SEG], F32)
    r_w = sb("r_w", [2, SEG], F32); r_dec = sb("r_dec", [2, SEG], F32)
    Bc = sb("Bc", [2, 1], F32); Gc = sb("Gc", [2, 1], F32)
    b_g = Buf("gates")
    rep = sb("rep", [128, 2, 4, SEG], F32); b_rep = Buf("rep")
    tokS = sb("tokS", [128, 16], F32); b_tokS = Buf("tokS")
    Cf = sb("Cf", [128, 2, 2, 512], F32); Cb = sb("Cb", [128, 2, 2, 512], BF16); b_C = Buf("C")
    nf = sb("nf", [128, 2, 2, 128], F32); nb = sb("nb", [128, 2, 2, 128], BF16); b_n = Buf("n")
    dT = sb("dT", [128, 128], F32); b_dT = Buf("dT")
    targ = sb("targ", [128, 128], F32); b_targ = Buf("targ")
    sdT = sb("sdT", [128, 128], BF16); b_sdT = Buf("sdT")
    qs = sb("qs", [128, 2, 128], BF16); b_qs = Buf("qs")
    rr = sb("rr", [128, 128], F32); b_rr = Buf("rr")
    kw = sb("kw", [128, 256], BF16); b_kw = Buf("kw")
    P.op("pool", lambda e: e.memset(Cf[:], 0.0), w=[b_C])
    P.op("pool", lambda e: e.memset(Cb[:], 0.0), w=[b_C])
    P.op("pool", lambda e: e.memset(nf[:], 0.0), w=[b_n])
    P.op("pool", lambda e: e.memset(nb[:], 0.0), w=[b_n])
    P.op("pool", lambda e: e.memset(Bc[:], 0.0), w=[b_g])
    P.op("pool", lambda e: e.memset(Gc[:], 0.0), w=[b_g])
    b_y = Buf("yout")
    wk = [0]

    def load_w(src_fn):
        k = wk[0] % 2
        wk[0] += 1
        P.dma("pool", lambda: wb[k][:], src_fn, w=[b_wb[k]])
        return wb[k], b_wb[k]

    for s in range(nseg):
        t0 = s * SEG
        for half in range(2):
            P.dma("sp", lambda half=half: hseg[:, half * 8:(half + 1) * 8, :],
                  lambda half=half: hT_full[half * 1024:(half + 1) * 1024, t0:t0 + SEG].rearrange("(c p) t -> p c t", p=128),
                  w=[b_hseg])
        rms_stats(P, cx, lambda c: hseg[:, c, :], DC, SEG, 7, sq, b_sq, b_hseg, rstd, b_rstd, 1.0 / D)
        for c in range(DC):
            P.op("dve", lambda e, c=c: e.scalar_tensor_tensor(out=xn[:, c, :], in0=hseg[:, c, :], scalar=gv[:, 0, c:c + 1],
                                                             in1=rstd[:, :], op0=ALU.mult, op1=ALU.mult),
                 r=[b_hseg, b_c2, b_rstd], w=[b_xn])
        for gi_, (gt, col) in enumerate(((gi, 0), (gf, 2))):
            ps = proj_fm(P, cx, wif_sb, col, xn, b_c2, b_xn, 6, ncol=2)
            P.op("act", lambda e, gt=gt, ps=ps, gi_=gi_: e.activation(out=gt[:, :], in_=ps[0:2, :], func=AF.Tanh,
                                                                       bias=gb15[:, gi_:gi_ + 1], scale=1.0 / 15.0),
                 r=[cx.bps[6], b_c2], w=[b_g])
        P.op("dve", lambda e: e.tensor_scalar_mul(out=gi[:, :], in0=gi[:, :], scalar1=15.0),
             r=[b_g], w=[b_g])
        P.op("act", lambda e: e.activation(out=gf[:, :], in_=gf[:, :], func=AF.Exp, scale=-15.0), r=[b_g], w=[b_g])
        P.op("act", lambda e: e.activation(out=gf[:, :], in_=gf[:, :], func=AF.Ln, bias=one_t[0:2, 0:1], scale=1.0),
             r=[b_g, cx.b_const], w=[b_g])
        P.op("dve", lambda e: e.tensor_scalar_mul(out=gf[:, :], in0=gf[:, :], scalar1=-1.0),
             r=[b_g], w=[b_g])
        P.op("dve", lambda e: e.tensor_tensor_scan(out=Bt[:, :], data0=ones_f[:, :], data1=gf[:, :], initial=Bc[:, 0:1],
                                                   op0=ALU.mult, op1=ALU.add), r=[b_g, b_c2], w=[b_g])
        P.op("dve", lambda e: e.tensor_tensor(out=al[:, :], in0=gi[:, :], in1=Bt[:, :], op=ALU.subtract), r=[b_g], w=[b_g])
        P.op("dve", lambda e: e.tensor_tensor_scan(out=Gt[:, :], data0=ones_f[:, :], data1=al[:, :], initial=Gc[:, 0:1],
                                                   op0=ALU.mult, op1=ALU.max), r=[b_g, b_c2], w=[b_g])
        for c in range(SEG // 128):
            cs = slice(c * 128, (c + 1) * 128)
            prev = Gc[:, 0:1] if c == 0 else Gt[:, c * 128 - 1:c * 128]
            P.op("dve", lambda e, cs=cs, prev=prev: e.tensor_scalar_mul(out=Gp[:, cs], in0=ones_f[:, cs], scalar1=prev), r=[b_g, b_c2], w=[b_g])
            P.op("dve", lambda e, cs=cs, c=c: e.tensor_scalar_mul(out=Gl[:, cs], in0=ones_f[:, cs],
                                                                 scalar1=Gt[:, c * 128 + 127:c * 128 + 128]), r=[b_g, b_c2], w=[b_g])
        P.op("dve", lambda e: e.tensor_tensor(out=r_int[:, :], in0=Gp[:, :], in1=Gt[:, :], op=ALU.subtract), r=[b_g], w=[b_g])
        P.op("dve", lambda e: e.tensor_tensor(out=r_em[:, :], in0=Bt[:, :], in1=Gt[:, :], op=ALU.add), r=[b_g], w=[b_g])
        P.op("dve", lambda e: e.tensor_tensor(out=r_w[:, :], in0=al[:, :], in1=Gl[:, :], op=ALU.subtract), r=[b_g], w=[b_g])
        P.op("dve", lambda e: e.tensor_tensor(out=r_dec[:, :], in0=Gp[:, :], in1=Gl[:, :], op=ALU.subtract), r=[b_g], w=[b_g])
        P.op("act", lambda e: e.activation(out=r_int[:, :], in_=r_int[:, :], func=AF.Exp), r=[b_g], w=[b_g])
        P.op("act", lambda e: e.activation(out=r_em[:, :], in_=r_em[:, :], func=AF.Exp, scale=-1.0), r=[b_g], w=[b_g])
        P.op("act", lambda e: e.activation(out=r_w[:, :], in_=r_w[:, :], func=AF.Exp), r=[b_g], w=[b_g])
        P.op("act", lambda e: e.activation(out=r_dec[:, :], in_=r_dec[:, :], func=AF.Exp), r=[b_g], w=[b_g])
        P.op("dve", lambda e: e.tensor_copy(out=Bc[:, :], in_=Bt[:, SEG - 1:SEG]), r=[b_g], w=[b_g])
        P.op("dve", lambda e: e.tensor_copy(out=Gc[:, :], in_=Gt[:, SEG - 1:SEG]), r=[b_g], w=[b_g])
        for h in range(2):
            for qi, src in enumerate((Gt, r_int, r_em, r_dec)):
                ps = cx.psum[6]
                P.op("pe", lambda e, h=h, src=src, ps=ps: e.matmul(ps[:, :], sel[0:2, h * 128:(h + 1) * 128], src[:, :],
                                                                   start=True, stop=True),
                     r=[b_g, b_c2], w=[cx.bps[6]])
                P.op("act", lambda e, h=h, qi=qi, ps=ps: e.activation(out=rep[:, h, qi, :], in_=ps[:, :], func=AF.Copy),
                     r=[cx.bps[6]], w=[b_rep])
        ps = cx.psum[6]
        for tq in range(4):
            for qi, src in enumerate((al, r_w)):
                P.op("pe", lambda e, tq=tq, qi=qi, src=src, ps=ps: e.matmul(
                    ps[:, tq * 4 + qi * 2:tq * 4 + qi * 2 + 2], src[0:2, tq * 128:(tq + 1) * 128], cx.ident_f[0:2, 0:2],
                    start=True, stop=True), r=[b_g, cx.b_const], w=[cx.bps[6]])
        P.op("dve", lambda e, ps=ps: e.tensor_copy(out=tokS[:, :], in_=ps[:, 0:16]), r=[cx.bps[6]], w=[b_tokS])
        wt, bw = load_w(lambda: wqk[0])
        for m in range(4):
            ps = proj_fm(P, cx, wt, m * 128, xn, bw, b_xn, m % 2)
            P.op("act", lambda e, m=m, ps=ps: e.activation(out=qT[:, m, :], in_=ps[:, :], func=AF.Copy,
                                                         scale=float(ML_DQK ** -0.5)), r=[cx.bps[m % 2]], w=[b_qT])
        wt, bw = load_w(lambda: wqk[1])
        for m in range(4):
            ps = proj_fm(P, cx, wt, m * 128, xn, bw, b_xn, m % 2)
            P.op("dve", lambda e, m=m, ps=ps: e.tensor_copy(out=kT[:, m, :], in_=ps[:, :]), r=[cx.bps[m % 2]], w=[b_kT])
        for tq in range(4):
            ps = cx.psum[tq % 2]
            for c in range(DC):
                P.op("pe", lambda e, c=c, tq=tq, ps=ps, wt=wt: e.matmul(ps[:, :], xn[:, c, tq * 128:(tq + 1) * 128], wt[:, c, :],
                                                                       start=(c == 0), stop=(c == DC - 1)),
                     r=[bw, b_xn], w=[cx.bps[tq % 2]])
            P.op("act", lambda e, tq=tq, ps=ps: e.activation(out=ktok[:, tq, :], in_=ps[:, :], func=AF.Copy),
                 r=[cx.bps[tq % 2]], w=[b_ktok])
        for h in range(2):
            wt, bw = load_w(lambda h=h: wv[h])
            for tq in range(4):
                ps = cx.psum[tq % 2]
                for c in range(DC):
                    P.op("pe", lambda e, c=c, tq=tq, ps=ps, wt=wt: e.matmul(ps[:, :], xn[:, c, tq * 128:(tq + 1) * 128],
                                                                           wt[:, c, :], start=(c == 0), stop=(c == DC - 1)),
                         r=[bw, b_xn], w=[cx.bps[tq % 2]])
                P.op("dve", lambda e, tq=tq, h=h, ps=ps: e.tensor_copy(out=vtok[:, tq, h, :], in_=ps[:, :]),
                     r=[cx.bps[tq % 2]], w=[b_vtok])
        for h in range(2):
            wt, bw = load_w(lambda h=h: wo[h])
            for m in range(4):
                ps = proj_fm(P, cx, wt, m * 128, xn, bw, b_xn, m % 2)
                P.op("act", lambda e, m=m, h=h, ps=ps: e.activation(out=og[:, h * 4 + m, :], in_=ps[:, :], func=AF.Sigmoid),
                     r=[cx.bps[m % 2]], w=[b_og])
        for tq in range(4):
            ts_ = slice(tq * 128, (tq + 1) * 128)
            for h in range(2):
                pS = cx.psum[2]
                for dc in range(2):
                    P.op("pe", lambda e, dc=dc, h=h, ts_=ts_, pS=pS: e.matmul(pS[:, 0:128], kT[:, 2 * h + dc, ts_],
                                                                             qT[:, 2 * h + dc, ts_], start=(dc == 0),
                                                                             stop=(dc == 1)),
                         r=[b_kT, b_qT], w=[cx.bps[2]])
                P.op("pool", lambda e, h=h, ts_=ts_: e.tensor_tensor(out=targ[:, :], in0=maskb[:, :], in1=rep[:, h, 0, ts_],
                                                                    op=ALU.subtract), r=[b_c2, b_rep], w=[b_targ])
                P.op("act", lambda e, h=h, tq=tq: e.activation(out=dT[:, :], in_=targ[:, :], func=AF.Exp,
                                                              bias=tokS[:, tq * 4 + h:tq * 4 + h + 1], scale=1.0),
                     r=[b_targ, b_tokS], w=[b_dT])
                P.op("dve", lambda e, pS=pS: e.tensor_tensor(out=sdT[:, :], in0=dT[:, :], in1=pS[:, 0:128], op=ALU.mult),
                     r=[b_dT, cx.bps[2]], w=[b_sdT])
                for dc in range(2):
                    P.op("pool", lambda e, dc=dc, h=h, ts_=ts_: e.tensor_tensor(out=qs[:, dc, :], in0=qT[:, 2 * h + dc, ts_],
                                                                               in1=rep[:, h, 1, ts_], op=ALU.mult),
                         r=[b_qT, b_rep], w=[b_qs])
                pH = cx.psum[3]
                for e_ in range(4):
                    es_ = slice(e_ * 128, (e_ + 1) * 128)
                    for dc in range(2):
                        P.op("pe", lambda e, dc=dc, h=h, es_=es_, pH=pH: e.matmul(pH[:, es_], Cb[:, h, dc, es_], qs[:, dc, :],
                                                                                 start=(dc == 0), stop=False),
                             r=[b_C, b_qs], w=[cx.bps[3]])
                    P.op("pe", lambda e, h=h, tq=tq, es_=es_, pH=pH: e.matmul(pH[:, es_], vtok[:, tq, h, es_], sdT[:, :],
                                                                             start=False, stop=True),
                         r=[b_vtok, b_sdT], w=[cx.bps[3]])
                pD = cx.psum[4]
                for dc in range(2):
                    P.op("pe", lambda e, dc=dc, h=h, pD=pD: e.matmul(pD[:, 0:128], nb[:, h, dc, :], qs[:, dc, :],
                                                                    start=(dc == 0), stop=False),
                         r=[b_n, b_qs], w=[cx.bps[4]])
                P.op("pe", lambda e, pD=pD: e.matmul(pD[:, 0:128], cx.ones_bf[:, :], sdT[:, :], start=False, stop=True),
                     r=[cx.b_const, b_sdT], w=[cx.bps[4]])
                P.op("act", lambda e, pD=pD: e.activation(out=rr[:, :], in_=pD[:, 0:128], func=AF.Abs), r=[cx.bps[4]], w=[b_rr])
                P.op("dve", lambda e, h=h, ts_=ts_: e.tensor_tensor(out=rr[:, :], in0=rr[:, :], in1=rep[:, h, 2, ts_],
                                                                   op=ALU.max), r=[b_rr, b_rep], w=[b_rr])
                P.op("dve", lambda e: e.reciprocal(out=rr[:, :], in_=rr[:, :]), r=[b_rr], w=[b_rr])
                for e_ in range(4):
                    es_ = slice(e_ * 128, (e_ + 1) * 128)
                    P.op("dve", lambda e, e_=e_, h=h, ts_=ts_, es_=es_, pH=pH: e.tensor_tensor(
                        out=hn[:, h * 4 + e_, ts_], in0=pH[:, es_], in1=rr[:, :], op=ALU.mult),
                        r=[cx.bps[3], b_rr], w=[b_hn])
                P.op("act", lambda e, h=h, tq=tq: e.activation(out=kw[:, :], in_=ktok[:, tq, h * 256:(h + 1) * 256], func=AF.Copy,
                                                              scale=tokS[:, tq * 4 + 2 + h:tq * 4 + 3 + h]),
                     r=[b_ktok, b_tokS], w=[b_kw])
                for dc in range(2):
                    pC = cx.psum[5 + dc]
                    P.op("pe", lambda e, dc=dc, h=h, tq=tq, pC=pC: e.matmul(pC[:, :], kw[:, dc * 128:(dc + 1) * 128],
                                                                           vtok[:, tq, h, :], start=True, stop=True),
                         r=[b_kw, b_vtok], w=[cx.bps[5 + dc]])
                    P.op("dve", lambda e, dc=dc, h=h, tq=tq, pC=pC: e.scalar_tensor_tensor(
                        out=Cf[:, h, dc, :], in0=Cf[:, h, dc, :], scalar=rep[:, h, 3, tq * 128:tq * 128 + 1], in1=pC[:, :],
                        op0=ALU.mult, op1=ALU.add), r=[cx.bps[5 + dc], b_rep], w=[b_C])
                    P.op("act", lambda e, dc=dc, h=h: e.activation(out=Cb[:, h, dc, :], in_=Cf[:, h, dc, :], func=AF.Copy),
                         r=[], w=[b_C])
                pN = cx.psum[7]
                for dc in range(2):
                    P.op("pe", lambda e, dc=dc, pN=pN: e.matmul(pN[:, dc * 128:(dc + 1) * 128], kw[:, dc * 128:(dc + 1) * 128],
                                                                cx.ones_bf[:, :], start=True, stop=True),
                         r=[b_kw, cx.b_const], w=[cx.bps[7]])
                for dc in range(2):
                    P.op("dve", lambda e, dc=dc, h=h, tq=tq, pN=pN: e.scalar_tensor_tensor(
                        out=nf[:, h, dc, :], in0=nf[:, h, dc, :], scalar=rep[:, h, 3, tq * 128:tq * 128 + 1],
                        in1=pN[:, dc * 128:(dc + 1) * 128], op0=ALU.mult, op1=ALU.add), r=[cx.bps[7], b_rep], w=[b_n])
                    P.op("act", lambda e, dc=dc, h=h: e.activation(out=nb[:, h, dc, :], in_=nf[:, h, dc, :], func=AF.Copy),
                         r=[], w=[b_n])
        for h in range(2):
            rms_stats(P, cx, lambda c, h=h: hn[:, h * 4 + c, :], 4, SEG, 7, sq, b_sq, b_hn, rstd, b_rstd, 1.0 / ML_DV)
            for e_ in range(4):
                P.op("dve", lambda e, e_=e_, h=h: e.scalar_tensor_tensor(out=hn[:, h * 4 + e_, :], in0=hn[:, h * 4 + e_, :],
                                                                        scalar=hg[:, h * 4 + e_:h * 4 + e_ + 1], in1=rstd[:, :],
                                                                        op0=ALU.mult, op1=ALU.mult),
                     r=[b_rstd, b_c2], w=[b_hn])
                P.op("pool", lambda e, e_=e_, h=h: e.tensor_tensor(out=hn[:, h * 4 + e_, :], in0=hn[:, h * 4 + e_, :],
                                                                  in1=og[:, h * 4 + e_, :], op=ALU.mult),
                     r=[b_og], w=[b_hn])
        P.dma("sp", lambda: yT[:, t0:t0 + SEG].rearrange("(c p) t -> p c t", p=128), lambda: hn[:, :, :], r=[b_hn], w=[b_y])
    P.finish()
    P.pop()
    P.es.close()


def consts2_np():
    c = np.zeros((128, 384), np.float32)
    s = np.arange(128)[:, None]
    l = np.arange(128)[None, :]
    c[:, 0:128] = np.where(s <= l, 0.0, NEG)
    c[0, 128:256] = 1.0
    c[1, 256:384] = 1.0
    return c


def lay_cols(w):
    return np.ascontiguousarray(w.reshape(DC, 128, -1).transpose(1, 0, 2))


GD_HK = 8
GD_HV = 16
NDT = F32


class Reg:
    def __init__(self, ap_fn, name):
        self.ap = ap_fn
        self.b = Buf(name)


def build_gdn_program(P, nseg=SEQ // SEG, stop=None):
    try:
        _build_gdn_body(P, nseg, stop)
    except StopBuild:
        P.finish()
        P.pop()
        P.es.close()


def _build_gdn_body(P, nseg, stop):
    cx = Ctx()
    hT_full = P.dram("hT_full", [D, SEQ], F32, kind="ExternalInput")
    consts = P.dram("consts", [128, 256], F32, kind="ExternalInput")
    consts2 = P.dram("consts2", [128, 384], F32, kind="ExternalInput")
    consts3 = P.dram("consts3", [128, 128], F32, kind="ExternalInput")
    gains = P.dram("gains", [128, 1, DC], F32, kind="ExternalInput")
    wq = P.dram("wq", [2, 128, DC, 512], F32, kind="ExternalInput")
    wk_ = P.dram("wk", [2, 128, DC, 512], F32, kind="ExternalInput")
    wv = P.dram("wv", [4, 128, DC, 512], F32, kind="ExternalInput")
    wz = P.dram("wz", [4, 128, DC, 512], F32, kind="ExternalInput")
    wba = P.dram("wba", [128, DC, 32], F32, kind="ExternalInput")
    convw = P.dram("convw", [128, 32, 4], F32, kind="ExternalInput")
    hpar = P.dram("hpar", [16, 2], F32, kind="ExternalInput")
    ng_in = P.dram("normg", [128, 1], F32, kind="ExternalInput")
    yT = P.dram("yT", [2048, SEQ], F32, kind="ExternalOutput")
    setup_common(P, cx, consts, npsum=7)
    sb = P.sb
    maskU = sb("maskU", [128, 128], F32)
    maskL = sb("maskL", [128, 128], F32)
    rsel = sb("rsel", [16, SEG], F32); b_rsel = Buf("rsel")
    ones_f = sb("ones_f", [16, SEG], F32)
    gv = sb("gv", [128, 1, DC], F32)
    wba_sb = sb("wba", [128, DC, 32], BF16)
    cw = sb("cw", [128, 32, 4], F32)
    hp = sb("hp", [16, 2], F32)
    nea = sb("nea", [16, 1], F32)
    ngt = sb("ngt", [128, 1], F32)
    b_c2 = Buf("c2")
    P.dma("sp", lambda: maskU[:], lambda: consts2[:, 0:128], w=[b_c2])
    P.dma("sp", lambda: maskL[:], lambda: consts3[:, 0:128], w=[b_c2])
    P.dma("sp", lambda: gv[:], lambda: gains[:], w=[b_c2])
    P.dma("pool", lambda: wba_sb[:], lambda: wba[:], w=[b_c2])
    P.dma("sp", lambda: cw[:], lambda: convw[:], w=[b_c2])
    P.dma("sp", lambda: hp[:], lambda: hpar[:], w=[b_c2])
    P.dma("sp", lambda: ngt[:], lambda: ng_in[:], w=[b_c2])
    P.op("pool", lambda e: e.memset(ones_f[:], 1.0), w=[b_c2])
    P.op("act", lambda e: e.activation(out=nea[:, :], in_=hp[:, 0:1], func=AF.Exp), r=[b_c2], w=[b_c2])
    P.op("dve", lambda e: e.tensor_scalar_mul(out=nea[:, :], in0=nea[:, :], scalar1=-1.0), r=[b_c2], w=[b_c2])
    one_t = cx.bias_tile(P, 1.0)

    hso = sb("hso", [128, DC, SEG], F32); b_hso = Buf("hso")
    xn = sb("xn", [128, DC, SEG], BF16); b_xn = Buf("xn")
    sq = [sb(f"sq{i}", [128, SEG], BF16) for i in range(2)]; b_sq = [Buf("sq0"), Buf("sq1")]
    rstd = sb("rstd", [128, SEG], F32); b_rstd = Buf("rstd")
    wb = [sb(f"wb{i}", [128, DC, 512], BF16) for i in range(2)]; b_wb = [Buf(f"wb{i}") for i in range(2)]
    qT = sb("qT", [128, GD_HK, SEG], BF16); b_qT = Buf("qT")
    kT = sb("kT", [128, GD_HK, SEG], BF16); b_kT = Buf("kT")
    vT = sb("vT", [128, GD_HV, SEG], BF16); b_vT = Buf("vT")
    zT = sb("zT", [128, GD_HV, SEG], BF16); b_zT = Buf("zT")
    hist = sb("hist", [128, 32, 3], F32); b_hist = Buf("hist")
    xc = [sb(f"xc{i}", [128, SEG + 3], F32) for i in range(2)]; b_xc = [Buf("xc0"), Buf("xc1")]
    co = [sb(f"co{i}", [128, SEG], F32) for i in range(2)]; b_co = [Buf("co0"), Buf("co1")]
    rb = sb("rb", [16, SEG], F32); rnb = sb("rnb", [16, SEG], F32)
    rg = sb("rg", [16, SEG], F32); rng_ = sb("rng", [16, SEG], F32)
    rt = sb("rt", [16, SEG], F32); rt2 = sb("rt2", [16, SEG], F32)
    rbg = sb("rbg", [16, SEG], F32); rkd = sb("rkd", [16, SEG], F32)
    b_g = Buf("gates")
    tokS = sb("tokS", [128, 4, 6, 16], F32); b_tokS = Buf("tokS")
    repg = [sb(f"repg{i}", [128, SEG], F32) for i in range(2)]; b_repg = [Buf("repg0"), Buf("repg1")]
    eg = [sb(f"eg{i}", [128, SEG], F32) for i in range(2)]; b_eg = [Buf("eg0"), Buf("eg1")]
    Sf = sb("Sf", [128, GD_HV, 128], F32); Sb = sb("Sb", [128, GD_HV, 128], BF16)
    b_S = [Buf(f"S{i}") for i in range(GD_HV)]
    t128 = lambda name, dt: sb(name, [128, 128], dt)
    targL = t128("targL", F32); targU = t128("targU", F32); decL = t128("decL", F32); decU = t128("decU", F32)
    b_tL, b_tU, b_dL, b_dU = Buf("tL"), Buf("tU"), Buf("dL"), Buf("dU")
    Xs = [t128("X0", NDT), t128("X1", NDT)]; Ys = [t128("Y0", NDT), t128("Y1", NDT)]
    b_X = [Buf("X0"), Buf("X1")]; b_Y = [Buf("Y0"), Buf("Y1")]
    Tt = t128("Tt", NDT); b_T = Buf("T")
    Tb = t128("Tb", BF16); b_Tb = Buf("Tb")
    QKd = t128("QKd", BF16); b_QKd = Buf("QKd")
    vb = t128("vb", BF16); b_vb = Buf("vb")
    kbg = t128("kbg", BF16); b_kbg = Buf("kbg")
    kdec = t128("kdec", BF16); b_kdec = Buf("kdec")
    nwT = t128("nwT", BF16); b_nwT = Buf("nwT")
    vnew = t128("vnew", BF16); b_vnew = Buf("vnew")
    qg = t128("qg", BF16); b_qg = Buf("qg")
    for i in range(GD_HV):
        P.op("pool", lambda e, i=i: e.memset(Sf[:, i, :], 0.0), w=[b_S[i]])
        P.op("pool", lambda e, i=i: e.memset(Sb[:, i, :], 0.0), w=[b_S[i]])
    P.op("pool", lambda e: e.memset(hist[:], 0.0), w=[b_hist])
    psb = P.ps("psb", [128, 1024], BF16)
    pf = cx.psum
    b_psb = Buf("psb", excl=True)

    def mkreg(fn, b):
        r = Reg(fn, "r")
        r.b = b
        return r

    R_KK = mkreg(lambda: pf[2][:, 0:128], cx.bps[2]); R_QK = mkreg(lambda: pf[2][:, 128:256], cx.bps[2])
    R_NA = [mkreg(lambda i=i: pf[3][:, i * 128:(i + 1) * 128], cx.bps[3]) for i in range(4)]
    R_NB = [mkreg(lambda i=i: pf[4][:, i * 128:(i + 1) * 128], cx.bps[4]) for i in range(4)]
    R_XT = mkreg(lambda: pf[5][:, 0:128], cx.bps[5]); R_SU = mkreg(lambda: pf[5][:, 0:128], cx.bps[5])
    R_W = mkreg(lambda: pf[5][:, 128:256], cx.bps[5]); R_VN = mkreg(lambda: pf[5][:, 256:384], cx.bps[5])
    R_O = mkreg(lambda: pf[5][:, 384:512], cx.bps[5])
    R_KT = mkreg(lambda: psb[:, 0:128], b_psb); R_VT = mkreg(lambda: psb[:, 128:256], b_psb)
    b_y = Buf("yout")
    wkc = [0]
    nrot = [0]

    def load_w(src_fn):
        k = wkc[0] % 2
        wkc[0] += 1
        P.dma("pool", lambda: wb[k][:], src_fn, w=[b_wb[k]])
        return wb[k], b_wb[k]

    def nreg(bank_a=False):
        r = (R_NA if bank_a else R_NB)[nrot[0] % 4]
        nrot[0] += 1
        return r

    for s in range(nseg):
        t0 = s * SEG
        for half in range(2):
            P.dma("sp", lambda half=half: hso[:, half * 8:(half + 1) * 8, :],
                  lambda half=half: hT_full[half * 1024:(half + 1) * 1024, t0:t0 + SEG].rearrange("(c p) t -> p c t", p=128),
                  w=[b_hso])
        rms_stats(P, cx, lambda c: hso[:, c, :], DC, SEG, 6, sq, b_sq, b_hso, rstd, b_rstd, 1.0 / D)
        for c in range(DC):
            P.op("dve", lambda e, c=c: e.scalar_tensor_tensor(out=xn[:, c, :], in0=hso[:, c, :], scalar=gv[:, 0, c:c + 1],
                                                             in1=rstd[:, :], op0=ALU.mult, op1=ALU.mult),
                 r=[b_hso, b_c2, b_rstd], w=[b_xn])
        ps = proj_fm(P, cx, wba_sb, 0, xn, b_c2, b_xn, 6, ncol=16)
        P.op("act", lambda e, ps=ps: e.activation(out=rb[:, :], in_=ps[0:16, :], func=AF.Sigmoid), r=[cx.bps[6]], w=[b_g])
        ps = proj_fm(P, cx, wba_sb, 16, xn, b_c2, b_xn, 6, ncol=16)
        P.op("act", lambda e, ps=ps: e.activation(out=rt[:, :], in_=ps[0:16, :], func=AF.Identity, bias=hp[:, 1:2], scale=1.0),
             r=[cx.bps[6], b_c2], w=[b_g])
        P.op("act", lambda e: e.activation(out=rt2[:, :], in_=rt[:, :], func=AF.Abs), r=[b_g], w=[b_g])
        P.op("act", lambda e: e.activation(out=rt2[:, :], in_=rt2[:, :], func=AF.Exp, scale=-1.0), r=[b_g], w=[b_g])
        P.op("act", lambda e: e.activation(out=rt2[:, :], in_=rt2[:, :], func=AF.Ln, bias=one_t[0:16, 0:1], scale=1.0),
             r=[b_g, cx.b_const], w=[b_g])
        P.op("dve", lambda e: e.tensor_scalar_max(out=rt[:, :], in0=rt[:, :], scalar1=0.0), r=[b_g], w=[b_g])
        P.op("dve", lambda e: e.tensor_tensor(out=rt[:, :], in0=rt[:, :], in1=rt2[:, :], op=ALU.add), r=[b_g], w=[b_g])
        P.op("dve", lambda e: e.tensor_scalar_mul(out=rt[:, :], in0=rt[:, :], scalar1=nea[:, 0:1]), r=[b_g, b_c2], w=[b_g])
        for c in range(SEG // 128):
            cs = slice(c * 128, (c + 1) * 128)
            P.op("dve", lambda e, cs=cs: e.tensor_tensor_scan(out=rg[:, cs], data0=ones_f[:, cs], data1=rt[:, cs], initial=0.0,
                                                              op0=ALU.mult, op1=ALU.add), r=[b_g, b_c2], w=[b_g])
            P.op("dve", lambda e, cs=cs, c=c: e.tensor_scalar_mul(out=rkd[:, cs], in0=ones_f[:, cs],
                                                                 scalar1=rg[:, c * 128 + 127:c * 128 + 128]),
                 r=[b_g, b_c2], w=[b_g])
        P.op("dve", lambda e: e.tensor_tensor(out=rkd[:, :], in0=rkd[:, :], in1=rg[:, :], op=ALU.subtract), r=[b_g], w=[b_g])
        P.op("act", lambda e: e.activation(out=rkd[:, :], in_=rkd[:, :], func=AF.Exp), r=[b_g], w=[b_g])
        P.op("act", lambda e: e.activation(out=rbg[:, :], in_=rg[:, :], func=AF.Exp), r=[b_g], w=[b_g])
        P.op("dve", lambda e: e.tensor_tensor(out=rbg[:, :], in0=rbg[:, :], in1=rb[:, :], op=ALU.mult), r=[b_g], w=[b_g])
        P.op("dve", lambda e: e.tensor_scalar_mul(out=rng_[:, :], in0=rg[:, :], scalar1=-1.0), r=[b_g], w=[b_g])
        P.op("dve", lambda e: e.tensor_scalar_mul(out=rnb[:, :], in0=rb[:, :], scalar1=-1.0), r=[b_g], w=[b_g])
        pt = cx.psum[6]
        for tq in range(4):
            for qi, src in enumerate((rg, rng_, rnb, rb, rbg, rkd)):
                col = (tq * 6 + qi) * 16
                P.op("pe", lambda e, tq=tq, src=src, col=col, pt=pt: e.matmul(pt[:, col:col + 16], src[0:16, tq * 128:(tq + 1) * 128],
                                                                            cx.ident_f[0:16, 0:16], start=True, stop=True),
                     r=[b_g, cx.b_const], w=[cx.bps[6]])
        P.op("dve", lambda e, pt=pt: e.tensor_copy(out=tokS[:].rearrange("p a b c -> p (a b c)"), in_=pt[:, 0:384]),
             r=[cx.bps[6]], w=[b_tokS])
        if stop == "gates":
            P.finish(); P.pop(); P.es.close()
            return
        for grp, (wsrc, nunit) in enumerate(((wq, 2), (wk_, 2), (wv, 4))):
            for u in range(nunit):
                wt, bw = load_w(lambda wsrc=wsrc, u=u: wsrc[u])
                for m in range(4):
                    cc = (0, 8, 16)[grp] + u * 4 + m
                    hh = u * 4 + m
                    ps = proj_fm(P, cx, wt, m * 128, xn, bw, b_xn, m % 2)
                    k = cc % 2
                    P.op("act", lambda e, ps=ps, k=k: e.activation(out=xc[k][:, 3:SEG + 3], in_=ps[:, :], func=AF.Copy),
                         r=[cx.bps[m % 2]], w=[b_xc[k]])
                    P.op("pool", lambda e, k=k, cc=cc: e.tensor_copy(out=xc[k][:, 0:3], in_=hist[:, cc, :]), r=[b_hist], w=[b_xc[k]])
                    P.op("pool", lambda e, k=k, cc=cc: e.tensor_copy(out=hist[:, cc, :], in_=xc[k][:, SEG:SEG + 3]),
                         r=[b_xc[k]], w=[b_hist])
                    P.op("dve", lambda e, k=k, cc=cc: e.tensor_scalar_mul(out=co[k][:, :], in0=xc[k][:, 0:SEG],
                                                                         scalar1=cw[:, cc, 0:1]), r=[b_xc[k], b_c2], w=[b_co[k]])
                    for j in range(1, 4):
                        P.op("dve", lambda e, k=k, cc=cc, j=j: e.scalar_tensor_tensor(
                            out=co[k][:, :], in0=xc[k][:, j:SEG + j], scalar=cw[:, cc, j:j + 1], in1=co[k][:, :],
                            op0=ALU.mult, op1=ALU.add), r=[b_xc[k], b_c2], w=[b_co[k]])
                    if grp == 2:
                        P.op("act", lambda e, k=k, hh=hh: e.activation(out=vT[:, hh, :], in_=co[k][:, :], func=AF.Silu),
                             r=[b_co[k]], w=[b_vT])
                    else:
                        dst, bd = (qT, b_qT) if grp == 0 else (kT, b_kT)
                        mul = 128.0 if grp == 0 else 1.0
                        P.op("act", lambda e, k=k: e.activation(out=co[k][:, :], in_=co[k][:, :], func=AF.Silu),
                             r=[], w=[b_co[k]])
                        P.op("act", lambda e, k=k: e.activation(out=sq[k][:, :], in_=co[k][:, :], func=AF.Square),
                             r=[b_co[k]], w=[b_sq[k]])
                        pn = cx.psum[6]
                        P.op("pe", lambda e, k=k, pn=pn: e.matmul(pn[:, :], cx.ones_bf[:, :], sq[k][:, :], start=True, stop=True),
                             r=[b_sq[k], cx.b_const], w=[cx.bps[6]])
                        bt = cx.bias_tile(P, EPS * mul)
                        P.op("act", lambda e, pn=pn, bt=bt, mul=mul: e.activation(out=rstd[:, :], in_=pn[:, :], func=AF.Sqrt,
                                                                               bias=bt[:, 0:1], scale=float(mul)),
                             r=[cx.bps[6], cx.b_const], w=[b_rstd])
                        P.op("dve", lambda e: e.reciprocal(out=rstd[:, :], in_=rstd[:, :]), r=[b_rstd], w=[b_rstd])
                        P.op("dve", lambda e, k=k, dst=dst, hh=hh: e.tensor_tensor(out=dst[:, hh, :], in0=co[k][:, :], in1=rstd[:, :],
                                                                                  op=ALU.mult), r=[b_co[k], b_rstd], w=[bd])
        for u in range(4):
            wt, bw = load_w(lambda u=u: wz[u])
            for m in range(4):
                ps = proj_fm(P, cx, wt, m * 128, xn, bw, b_xn, m % 2)
                P.op("act", lambda e, ps=ps, u=u, m=m: e.activation(out=zT[:, u * 4 + m, :], in_=ps[:, :], func=AF.Silu),
                     r=[cx.bps[m % 2]], w=[b_zT])
        if stop == "proj":
            P.finish(); P.pop(); P.es.close()
            return
        for hk in range(GD_HK):
            for hv in (2 * hk, 2 * hk + 1):
                ri = hv % 2
                pr = cx.psum[6]
                P.op("dve", lambda e, hv=hv: e.tensor_scalar_mul(out=rsel[:, :], in0=rg[:, :], scalar1=cx.ident_f[0:16, hv:hv + 1]),
                     r=[b_g, cx.b_const], w=[b_rsel])
                P.op("pe", lambda e, pr=pr: e.matmul(pr[:, :], ones_f[0:16, 0:128], rsel[:, :], start=True, stop=True),
                     r=[b_rsel, b_c2], w=[cx.bps[6]])
                P.op("act", lambda e, ri=ri, pr=pr: e.activation(out=repg[ri][:, :], in_=pr[:, :], func=AF.Copy),
                     r=[cx.bps[6]], w=[b_repg[ri]])
                P.op("act", lambda e, ri=ri, pr=pr: e.activation(out=eg[ri][:, :], in_=pr[:, :], func=AF.Exp),
                     r=[cx.bps[6]], w=[b_eg[ri]])
            for tq in range(4):
                ts_ = slice(tq * 128, (tq + 1) * 128)
                P.op("pe", lambda e, hk=hk, ts_=ts_: e.matmul(R_KK.ap(), kT[:, hk, ts_], kT[:, hk, ts_], start=True, stop=True),
                     r=[b_kT], w=[R_KK.b])
                P.op("pe", lambda e, hk=hk, ts_=ts_: e.matmul(R_QK.ap(), kT[:, hk, ts_], qT[:, hk, ts_], start=True, stop=True),
                     r=[b_kT, b_qT], w=[R_QK.b])
                P.op("pe", lambda e, hk=hk, ts_=ts_: e.transpose(R_KT.ap(), kT[:, hk, ts_], cx.ident_bf[:, :]),
                     r=[b_kT, cx.b_const], w=[R_KT.b])
                for hv in (2 * hk, 2 * hk + 1):
                    ri = hv % 2
                    S_g = lambda qi: tokS[:, tq, qi, hv:hv + 1]
                    P.op("pool", lambda e, ri=ri, ts_=ts_: e.tensor_tensor(out=targL[:, :], in0=maskL[:, :], in1=repg[ri][:, ts_],
                                                                          op=ALU.subtract), r=[b_c2, b_repg[ri]], w=[b_tL])
                    P.op("act", lambda e, S_g=S_g: e.activation(out=decL[:, :], in_=targL[:, :], func=AF.Exp, bias=S_g(0), scale=1.0),
                         r=[b_tL, b_tokS], w=[b_dL])
                    P.op("pool", lambda e, ri=ri, ts_=ts_: e.tensor_tensor(out=targU[:, :], in0=maskU[:, :], in1=repg[ri][:, ts_],
                                                                          op=ALU.add), r=[b_c2, b_repg[ri]], w=[b_tU])
                    P.op("act", lambda e, S_g=S_g: e.activation(out=decU[:, :], in_=targU[:, :], func=AF.Exp, bias=S_g(1), scale=1.0),
                         r=[b_tU, b_tokS], w=[b_dU])
                    P.op("dve", lambda e, S_g=S_g: e.scalar_tensor_tensor(out=Xs[0][:, :], in0=R_KK.ap(), scalar=S_g(2), in1=decL[:, :],
                                                                        op0=ALU.mult, op1=ALU.mult),
                         r=[R_KK.b, b_tokS, b_dL], w=[b_X[0]])
                    P.op("pe", lambda e: e.matmul(R_XT.ap(), Xs[0][:, :], cx.ident_f[:, :], start=True, stop=True),
                         r=[b_X[0], cx.b_const], w=[R_XT.b])
                    P.op("act", lambda e: e.activation(out=Ys[0][:, :], in_=R_XT.ap(), func=AF.Copy), r=[R_XT.b], w=[b_Y[0]])
                    P.op("pool", lambda e: e.tensor_tensor(out=Tt[:, :], in0=Ys[0][:, :], in1=cx.ident_f[:, :], op=ALU.add),
                         r=[b_Y[0], cx.b_const], w=[b_T])
                    P.op("dve", lambda e: e.tensor_tensor(out=QKd[:, :], in0=decU[:, :], in1=R_QK.ap(), op=ALU.mult),
                         r=[R_QK.b, b_dU], w=[b_QKd])
                    cur = 0
                    for lev in range(1, 7):
                        nxt = 1 - cur
                        r1 = nreg(True)
                        P.op("pe", lambda e, cur=cur, r1=r1: e.matmul(r1.ap(), Ys[cur][:, :], Xs[cur][:, :], start=True, stop=True),
                             r=[b_X[cur], b_Y[cur]], w=[r1.b])
                        if lev < 6:
                            r2 = nreg()
                            P.op("pe", lambda e, cur=cur, r2=r2: e.matmul(r2.ap(), Xs[cur][:, :], Ys[cur][:, :], start=True, stop=True),
                                 r=[b_X[cur], b_Y[cur]], w=[r2.b])
                        P.op("act", lambda e, nxt=nxt, r1=r1: e.activation(out=Xs[nxt][:, :], in_=r1.ap(), func=AF.Copy),
                             r=[r1.b], w=[b_X[nxt]])
                        if lev < 6:
                            P.op("dve", lambda e, nxt=nxt, r2=r2: e.tensor_copy(out=Ys[nxt][:, :], in_=r2.ap()), r=[r2.b], w=[b_Y[nxt]])
                        r3 = nreg()
                        P.op("pe", lambda e, nxt=nxt, r3=r3: e.matmul(r3.ap(), Xs[nxt][:, :], Tt[:, :], start=True, stop=True),
                             r=[b_X[nxt], b_T], w=[r3.b])
                        P.op("dve", lambda e, r3=r3: e.tensor_tensor(out=Tt[:, :], in0=Tt[:, :], in1=r3.ap(), op=ALU.add),
                             r=[r3.b], w=[b_T])
                        cur = nxt
                    P.op("act", lambda e: e.activation(out=Tb[:, :], in_=Tt[:, :], func=AF.Copy), r=[b_T], w=[b_Tb])
                    P.op("pe", lambda e, hv=hv, ts_=ts_: e.transpose(R_VT.ap(), vT[:, hv, ts_], cx.ident_bf[:, :]),
                         r=[b_vT, cx.b_const], w=[R_VT.b])
                    P.op("act", lambda e, S_g=S_g: e.activation(out=vb[:, :], in_=R_VT.ap(), func=AF.Copy, scale=S_g(3)),
                         r=[R_VT.b, b_tokS], w=[b_vb])
                    P.op("dve", lambda e, S_g=S_g: e.tensor_scalar_mul(out=kbg[:, :], in0=R_KT.ap(), scalar1=S_g(4)),
                         r=[R_KT.b, b_tokS], w=[b_kbg])
                    P.op("act", lambda e, S_g=S_g: e.activation(out=kdec[:, :], in_=R_KT.ap(), func=AF.Copy, scale=S_g(5)),
                         r=[R_KT.b, b_tokS], w=[b_kdec])
                    P.op("pe", lambda e: e.matmul(R_W.ap(), kbg[:, :], Tb[:, :], start=True, stop=True), r=[b_kbg, b_Tb], w=[R_W.b])
                    P.op("dve", lambda e: e.tensor_scalar_mul(out=nwT[:, :], in0=R_W.ap(), scalar1=-1.0), r=[R_W.b], w=[b_nwT])
                    P.op("pe", lambda e: e.matmul(R_VN.ap(), Tb[:, :], vb[:, :], start=True, stop=False), r=[b_Tb, b_vb], w=[R_VN.b])
                    P.op("pe", lambda e, hv=hv: e.matmul(R_VN.ap(), nwT[:, :], Sb[:, hv, :], start=False, stop=True),
                         r=[b_nwT, b_S[hv]], w=[R_VN.b])
                    P.op("act", lambda e: e.activation(out=vnew[:, :], in_=R_VN.ap(), func=AF.Copy), r=[R_VN.b], w=[b_vnew])
                    P.op("pool", lambda e, hk=hk, ri=ri, ts_=ts_: e.tensor_tensor(out=qg[:, :], in0=qT[:, hk, ts_], in1=eg[ri][:, ts_],
                                                                                 op=ALU.mult), r=[b_qT, b_eg[ri]], w=[b_qg])
                    P.op("pe", lambda e, hv=hv: e.matmul(R_O.ap(), Sb[:, hv, :], qg[:, :], start=True, stop=False),
                         r=[b_S[hv], b_qg], w=[R_O.b])
                    P.op("pe", lambda e: e.matmul(R_O.ap(), vnew[:, :], QKd[:, :], start=False, stop=True),
                         r=[b_vnew, b_QKd], w=[R_O.b])
                    P.op("dve", lambda e, hv=hv, ts_=ts_: e.tensor_copy(out=hso[:, hv, ts_], in_=R_O.ap()), r=[R_O.b], w=[b_hso])
                    P.op("pe", lambda e: e.matmul(R_SU.ap(), kdec[:, :], vnew[:, :], start=True, stop=True),
                         r=[b_kdec, b_vnew], w=[R_SU.b])
                    P.op("dve", lambda e, hv=hv, ri=ri, tq=tq: e.scalar_tensor_tensor(
                        out=Sf[:, hv, :], in0=Sf[:, hv, :], scalar=eg[ri][:, tq * 128 + 127:tq * 128 + 128], in1=R_SU.ap(),
                        op0=ALU.mult, op1=ALU.add), r=[R_SU.b, b_eg[ri]], w=[b_S[hv]])
                    P.op("act", lambda e, hv=hv: e.activation(out=Sb[:, hv, :], in_=Sf[:, hv, :], func=AF.Copy), r=[], w=[b_S[hv]])
        if stop == "chunks":
            P.finish(); P.pop(); P.es.close()
            return
        for hv in range(GD_HV):
            k = hv % 2
            P.op("act", lambda e, k=k, hv=hv: e.activation(out=sq[k][:, :], in_=hso[:, hv, :], func=AF.Square), r=[b_hso], w=[b_sq[k]])
            pn = cx.psum[6]
            P.op("pe", lambda e, k=k, pn=pn: e.matmul(pn[:, :], cx.ones_bf[:, :], sq[k][:, :], start=True, stop=True),
                 r=[b_sq[k], cx.b_const], w=[cx.bps[6]])
            bt = cx.bias_tile(P, EPS)
            P.op("act", lambda e, pn=pn, bt=bt: e.activation(out=rstd[:, :], in_=pn[:, :], func=AF.Sqrt, bias=bt[:, 0:1],
                                                           scale=1.0 / 128.0), r=[cx.bps[6], cx.b_const], w=[b_rstd])
            P.op("dve", lambda e: e.reciprocal(out=rstd[:, :], in_=rstd[:, :]), r=[b_rstd], w=[b_rstd])
            P.op("dve", lambda e, hv=hv: e.scalar_tensor_tensor(out=hso[:, hv, :], in0=hso[:, hv, :], scalar=ngt[:, 0:1], in1=rstd[:, :],
                                                               op0=ALU.mult, op1=ALU.mult), r=[b_rstd, b_c2], w=[b_hso])
            P.op("pool", lambda e, hv=hv: e.tensor_tensor(out=hso[:, hv, :], in0=hso[:, hv, :], in1=zT[:, hv, :], op=ALU.mult),
                 r=[b_zT], w=[b_hso])
        P.dma("sp", lambda: yT[:, t0:t0 + SEG].rearrange("(c p) t -> p c t", p=128), lambda: hso[:, :, :], r=[b_hso], w=[b_y])
    P.finish()
    P.pop()
    P.es.close()


def consts3_np():
    c = np.zeros((128, 128), np.float32)
    l = np.arange(128)[:, None]
    s = np.arange(128)[None, :]
    c[:, 0:128] = np.where(s < l, 0.0, NEG)
    return c


def units(w, n):
    return np.stack([lay_cols(w[:, i * 512:(i + 1) * 512]) for i in range(n)])


def ml_inputs(hT_full, g2, w_in, ib, fb, hg, hp):
    hs = [2 * hp, 2 * hp + 1]
    qc = np.concatenate([w_in[:, hh * 256:(hh + 1) * 256] for hh in hs], 1)
    kc = np.concatenate([w_in[:, 1024 + hh * 256:1024 + (hh + 1) * 256] for hh in hs], 1)
    wqk = np.stack([lay_cols(qc), lay_cols(kc)])
    wv = np.stack([lay_cols(w_in[:, 2048 + hh * 512:2048 + (hh + 1) * 512]) for hh in hs])
    wo = np.stack([lay_cols(w_in[:, 4096 + hh * 512:4096 + (hh + 1) * 512]) for hh in hs])
    wif = lay_cols(np.stack([w_in[:, 6144 + hs[0]], w_in[:, 6144 + hs[1]], w_in[:, 6148 + hs[0]], w_in[:, 6148 + hs[1]]], 1))
    gb = np.array([[ib[hs[0]], fb[hs[0]]], [ib[hs[1]], fb[hs[1]]]], np.float32)
    headg = np.ascontiguousarray(hg[hs].reshape(2, 4, 128).transpose(2, 0, 1).reshape(128, 8))
    return {"hT_full": hT_full, "consts": consts_np(), "consts2": consts2_np(), "gains": lay_gain(g2), "wqk": wqk,
            "wv": wv, "wo": wo, "wif": wif, "gbias": gb, "headg": headg}


def gdn_inputs(hT_full, g2, w_in, conv_w, a_log, dt_bias, norm_g, hp):
    qs = slice(hp * 1024, (hp + 1) * 1024)
    ks = slice(2048 + hp * 1024, 2048 + (hp + 1) * 1024)
    vs = slice(4096 + hp * 2048, 4096 + (hp + 1) * 2048)
    zs = slice(8192 + hp * 2048, 8192 + (hp + 1) * 2048)
    bs = slice(12288 + hp * 16, 12288 + (hp + 1) * 16)
    as_ = slice(12320 + hp * 16, 12320 + (hp + 1) * 16)
    cwm = np.concatenate([conv_w[:, qs], conv_w[:, ks], conv_w[:, vs]], 1)
    convw = np.ascontiguousarray(cwm.reshape(4, 32, 128).transpose(2, 1, 0))
    return {"hT_full": hT_full, "consts": consts_np(), "consts2": consts2_np(), "consts3": consts3_np(),
            "gains": lay_gain(g2), "wq": units(w_in[:, qs], 2), "wk": units(w_in[:, ks], 2), "wv": units(w_in[:, vs], 4),
            "wz": units(w_in[:, zs], 4), "wba": lay_cols(np.concatenate([w_in[:, bs], w_in[:, as_]], 1)), "convw": convw,
            "hpar": np.ascontiguousarray(np.stack([a_log[hp * 16:(hp + 1) * 16], dt_bias[hp * 16:(hp + 1) * 16]], 1)),
            "normg": np.ascontiguousarray(norm_g.reshape(128, 1))}


_PROG_CACHE = {}


def _prog(key, builder, *args):
    if key not in _PROG_CACHE:
        _PROG_CACHE[key] = make_program(builder, *args)
    return _PROG_CACHE[key]


def kernel(x, norm_g, ffn_w_in, ffn_w_out, ml_w_in, ml_i_bias, ml_f_bias, ml_head_g, ml_w_out,
           gdn_w_in, gdn_conv_w, gdn_a_log, gdn_dt_bias, gdn_norm_g, gdn_w_out):
    f = lambda a: np.asarray(a, dtype=np.float32)
    x, norm_g, ffn_w_in, ffn_w_out = f(x), f(norm_g), f(ffn_w_in), f(ffn_w_out)
    ml_w_in, ml_i_bias, ml_f_bias, ml_head_g, ml_w_out = f(ml_w_in), f(ml_i_bias), f(ml_f_bias), f(ml_head_g), f(ml_w_out)
    gdn_w_in, gdn_conv_w, gdn_a_log, gdn_dt_bias = f(gdn_w_in), f(gdn_conv_w), f(gdn_a_log), f(gdn_dt_bias)
    gdn_norm_g, gdn_w_out = f(gdn_norm_g), f(gdn_w_out)
    depth = norm_g.shape[0]
    cores = list(range(NCORES))
    gains = lay_gain(norm_g.reshape(depth * 6, D))
    cst = consts_np()
    hT = [np.ascontiguousarray(x[c // 2, (c % 2) * TOK:(c % 2 + 1) * TOK, :].T) for c in cores]

    def run_tok(stages, extra):
        nc = _prog(("tok", tuple(stages)), build_tok_program, list(stages))
        ims = []
        for c in cores:
            im = {"hT_in": hT[c], "consts": cst, "gains": gains}
            for k, v in extra.items():
                im[k] = v[c] if isinstance(v, list) else v
            ims.append(im)
        res = run_bass_kernel_spmd(nc, ims, core_ids=cores)
        return [res.results[c]["hT_out"] for c in cores]

    def ffn_w(layer, which, i):
        return {f"w_in_{i}": lay_w_in(ffn_w_in[layer, which]), f"w_out_{i}": lay_w_out(ffn_w_out[layer, which])}

    hT = run_tok([("ffn", 0, 1)], ffn_w(0, 0, 0))
    for layer in range(depth):
        j = layer // 2
        hfull = [np.ascontiguousarray(np.concatenate([hT[2 * b], hT[2 * b + 1]], axis=1)) for b in range(4)]
        g2 = norm_g[layer, 2:3]
        if layer % 2 == 0:
            nc = _prog(("ml",), build_mlstm_program)
            ims = [ml_inputs(hfull[c // 2], g2, ml_w_in[j], ml_i_bias[j], ml_f_bias[j], ml_head_g[j], c % 2) for c in cores]
            nk, wmix = 16, ml_w_out[j]
        else:
            nc = _prog(("gdn",), build_gdn_program)
            ims = [gdn_inputs(hfull[c // 2], g2, gdn_w_in[j], gdn_conv_w[j], gdn_a_log[j], gdn_dt_bias[j], gdn_norm_g[j], c % 2)
                   for c in cores]
            nk, wmix = 32, gdn_w_out[j]
        res = run_bass_kernel_spmd(nc, ims, core_ids=cores)
        yT = [res.results[c]["yT"] for c in cores]
        y_in = [np.ascontiguousarray(np.concatenate([yT[2 * (c // 2)][:, (c % 2) * TOK:(c % 2 + 1) * TOK],
                                                     yT[2 * (c // 2) + 1][:, (c % 2) * TOK:(c % 2 + 1) * TOK]], axis=0))
                for c in cores]
        stages = [("mix", layer * 6 + 3, nk), ("ffn", layer * 6 + 4, layer * 6 + 5)]
        extra = {"yT_0": y_in, "w_out_0": lay_w_out(wmix)}
        extra.update(ffn_w(layer, 1, 1))
        if layer + 1 < depth:
            stages.append(("ffn", (layer + 1) * 6 + 0, (layer + 1) * 6 + 1))
            extra.update(ffn_w(layer + 1, 0, 2))
        hT = run_tok(stages, extra)
    out = np.empty((4, SEQ, D), np.float32)
    for c in cores:
        out[c // 2, (c % 2) * TOK:(c % 2 + 1) * TOK, :] = hT[c].T
    return out
```
